# Optimizing a Trainium2 kernel written in Bass

```python
import math
import jax
import jax.numpy as jnp
from jax import lax
import numpy as np

D_MODEL = 1024
BATCH = 4
SEQ = 4096
DEPTH = 2

GRID_W = 64
HEAD_DIM = 64
QUERY_BLOCK = 128
ROPE_THETA = 500000.0
ROPE_FRACTION = 4
LN_EPS = 1e-5
NA_HEADS = 4
NA_ROWS = 8
NA_COLS = 16
DIFF_HEADS = 4
DIFF_QK_DIM = 32
DIFF_V_DIM = 2 * DIFF_QK_DIM
POOL_WINDOWS = (2, 4, 8, 16)
POOL_GROUP = 64
POOL_WIDTH = len(POOL_WINDOWS) * POOL_GROUP
DIL_HEADS = 4
DIL_PATTERNS = ((128, 1), (512, 4), (2048, 16))
N_BRANCHES = 4
BRANCH_WIDTH = 256
COLS_NA = 3 * NA_HEADS * HEAD_DIM
COLS_DIFF = 2 * DIFF_HEADS * 2 * DIFF_QK_DIM + DIFF_HEADS * DIFF_V_DIM
COLS_POOL = POOL_WIDTH
COLS_DIL = len(DIL_PATTERNS) * 3 * DIL_HEADS * HEAD_DIM
COLS_GATE = N_BRANCHES * D_MODEL
IN_SPLITS = (COLS_NA, COLS_NA + COLS_DIFF, COLS_NA + COLS_DIFF + COLS_POOL,
             COLS_NA + COLS_DIFF + COLS_POOL + COLS_DIL)
IN_COLS = IN_SPLITS[-1] + COLS_GATE
N_GROUPS = 4
EXPERTS_PER_GROUP = 8
N_EXPERTS = N_GROUPS * EXPERTS_PER_GROUP
TOP_K = 2
D_EXPERT = 512
MOE_BLOCK = 128
ALPHA = (2 * DEPTH) ** 0.25
BETA = (8 * DEPTH) ** -0.25

kernel_name = 'hybrid_gated_encoder_hmoe'


def layer_norm(x, g, b):
    xf = x.astype(jnp.float32)
    mu = jnp.mean(xf, axis=-1, keepdims=True)
    var = jnp.mean(jnp.square(xf - mu), axis=-1, keepdims=True)
    return ((xf - mu) * lax.rsqrt(var + LN_EPS) * g + b).astype(x.dtype)


def partial_rope(x, pos):
    rot = x.shape[-1] // ROPE_FRACTION
    half = rot // 2
    inv_freq = jnp.exp(jnp.arange(half, dtype=jnp.float32) * (-2.0 * math.log(ROPE_THETA) / rot))
    ang = pos.astype(jnp.float32)[:, None] * inv_freq[None, :]
    cos, sin = jnp.cos(ang), jnp.sin(ang)
    x1 = x[..., :half].astype(jnp.float32)
    x2 = x[..., half:rot].astype(jnp.float32)
    return jnp.concatenate([(x1 * cos - x2 * sin).astype(x.dtype),
                            (x1 * sin + x2 * cos).astype(x.dtype), x[..., rot:]], axis=-1)


def neighbourhood_attention(q, k, v, rpb):
    b, h, s, d = q.shape
    rows = s // GRID_W
    kr = min(NA_ROWS, rows)
    kc = NA_COLS
    qg = q.reshape(b, h, rows, GRID_W, d)
    kg = k.reshape(b, h, rows, GRID_W, d)
    vg = v.reshape(b, h, rows, GRID_W, d)
    r = jnp.arange(rows)
    row_idx = jnp.clip(r - kr // 2, 0, rows - kr)[:, None] + jnp.arange(kr)[None, :]
    k_rows = kg[:, :, row_idx]
    v_rows = vg[:, :, row_idx]
    sc = jnp.einsum('bhrqd,bhrkcd->bhrqkc', qg, k_rows).astype(jnp.float32) * d ** -0.5
    col = jnp.arange(GRID_W)
    col_start = jnp.clip(col - kc // 2, 0, GRID_W - kc)
    in_win = (col[None, :] >= col_start[:, None]) & (col[None, :] < col_start[:, None] + kc)
    dr = row_idx - r[:, None] + NA_ROWS - 1
    dc = jnp.clip(col[None, :] - col[:, None] + kc - 1, 0, 2 * kc - 2)
    bias = rpb[:, dr][..., dc].transpose(0, 1, 3, 2, 4).astype(jnp.float32)
    sc = jnp.where(in_win[:, None, :], sc + bias[None], -jnp.inf)
    p = jax.nn.softmax(sc, axis=(-2, -1))
    o = jnp.einsum('bhrqkc,bhrkcd->bhrqd', p.astype(v.dtype), v_rows)
    return o.reshape(b, h, s, d)


def differential_attention(q, k, v, lam, lam_init, subln_g):
    b, h, _, s, dq = q.shape
    nb = s // QUERY_BLOCK
    qb = q.reshape(b, h, 2, nb, QUERY_BLOCK, dq).transpose(3, 0, 1, 2, 4, 5)

    def block(qblk):
        sc = jnp.einsum('bhmqd,bhmkd->bhmqk', qblk, k).astype(jnp.float32) * dq ** -0.5
        p = jax.nn.softmax(sc, axis=-1)
        a = p[:, :, 0] - lam * p[:, :, 1]
        return jnp.einsum('bhqk,bhkd->bhqd', a.astype(v.dtype), v)

    o = lax.map(block, qb)
    o = o.transpose(1, 2, 0, 3, 4).reshape(b, h, s, -1).astype(jnp.float32)
    o = o * lax.rsqrt(jnp.mean(o * o, axis=-1, keepdims=True) + LN_EPS) * subln_g * (1.0 - lam_init)
    return o.astype(v.dtype)


def multiscale_pool(u, pool_w, pool_scale):
    b, s, _ = u.shape
    ng = len(POOL_WINDOWS)
    ug = u.reshape(b, s, ng, POOL_GROUP).astype(jnp.float32)
    cs = jnp.concatenate([jnp.zeros((b, 1, ng, POOL_GROUP), jnp.float32), jnp.cumsum(ug, axis=1)], axis=1)
    t = jnp.arange(s)
    outs = []
    for gi, w in enumerate(POOL_WINDOWS):
        lo = jnp.maximum(t - w // 2, 0)
        hi = jnp.minimum(t + w - 1 - w // 2, s - 1)
        csg = cs[:, :, gi]
        mean = (csg[:, hi + 1] - csg[:, lo]) / (hi - lo + 1).astype(jnp.float32)[:, None]
        outs.append(mean - ug[:, :, gi])
    dlt = jnp.stack(outs, axis=2).astype(u.dtype)
    y = jnp.einsum('bsgc,gce->bsge', dlt, pool_w).reshape(b, s, -1)
    return y * pool_scale


def dilated_pattern(q, k, v, window, dilation):
    b, h, s, d = q.shape
    half = window // (2 * dilation)
    length = s // dilation
    qb = math.gcd(QUERY_BLOCK, length)
    nb = length // qb
    band = qb + 2 * half

    def sub(x):
        return x.reshape(b, h, length, dilation, d).transpose(0, 1, 3, 2, 4)

    pad = ((0, 0), (0, 0), (0, 0), (half, half), (0, 0))
    kp = jnp.pad(sub(k), pad)
    vp = jnp.pad(sub(v), pad)
    idx = jnp.arange(nb)[:, None] * qb + jnp.arange(band)[None, :]
    kb = kp[:, :, :, idx]
    vb = vp[:, :, :, idx]
    qblk = sub(q).reshape(b, h, dilation, nb, qb, d)
    sc = jnp.einsum('bhmnqd,bhmnkd->bhmnqk', qblk, kb).astype(jnp.float32) * d ** -0.5
    a = jnp.arange(qb)[:, None]
    c = jnp.arange(band)[None, :]
    in_win = (c >= a) & (c <= a + 2 * half)
    in_seq = (idx >= half) & (idx < length + half)
    sc = jnp.where(in_win[None] & in_seq[:, None, :], sc, -jnp.inf)
    m = jnp.max(sc, axis=-1, keepdims=True)
    e = jnp.exp(sc - m)
    den = jnp.sum(e, axis=-1, keepdims=True)
    o = jnp.einsum('bhmnqk,bhmnkd->bhmnqd', (e / den).astype(v.dtype), vb)
    lse = (m + jnp.log(den))[..., 0]
    o = o.reshape(b, h, dilation, length, d).transpose(0, 1, 3, 2, 4).reshape(b, h, s, d)
    lse = lse.reshape(b, h, dilation, length).transpose(0, 1, 3, 2).reshape(b, h, s)
    return o, lse


def token_mixing(hs, pos, lam_init, w_in, b_gate, na_rpb, diff_lam, diff_subln_g,
                 pool_w, pool_scale, w_branch, w_out):
    b, s, dm = hs.shape
    z = hs @ w_in
    z_na, z_diff, z_pool, z_dil, z_gate = jnp.split(z, IN_SPLITS, axis=-1)

    def heads(t, n):
        return t.reshape(b, s, n, -1).transpose(0, 2, 1, 3)

    def unheads(t):
        return t.transpose(0, 2, 1, 3).reshape(b, s, -1).astype(hs.dtype)

    qa, ka, va = jnp.split(z_na, 3, axis=-1)
    y_a = unheads(neighbourhood_attention(heads(qa, NA_HEADS), heads(ka, NA_HEADS), heads(va, NA_HEADS), na_rpb))

    qkw = DIFF_HEADS * 2 * DIFF_QK_DIM
    qd = z_diff[..., :qkw].reshape(b, s, DIFF_HEADS, 2, DIFF_QK_DIM).transpose(0, 2, 3, 1, 4)
    kd = z_diff[..., qkw:2 * qkw].reshape(b, s, DIFF_HEADS, 2, DIFF_QK_DIM).transpose(0, 2, 3, 1, 4)
    vd = heads(z_diff[..., 2 * qkw:], DIFF_HEADS)
    dl = diff_lam.astype(jnp.float32)
    lam = jnp.exp(jnp.sum(dl[0] * dl[1])) - jnp.exp(jnp.sum(dl[2] * dl[3])) + lam_init
    y_b = unheads(differential_attention(partial_rope(qd, pos), partial_rope(kd, pos), vd,
                                         lam, lam_init, diff_subln_g))

    y_c = multiscale_pool(z_pool, pool_w, pool_scale).astype(hs.dtype)

    zd = z_dil.reshape(b, s, len(DIL_PATTERNS), 3, DIL_HEADS, HEAD_DIM).transpose(2, 3, 0, 4, 1, 5)
    outs, lses = [], []
    for pi, (w, r) in enumerate(DIL_PATTERNS):
        o, l = dilated_pattern(partial_rope(zd[pi, 0], pos), partial_rope(zd[pi, 1], pos), zd[pi, 2], w, r)
        outs.append(o)
        lses.append(l)
    o_all = jnp.stack(outs)
    w_all = jax.nn.softmax(jnp.stack(lses), axis=0)
    y_d = jnp.einsum('pbhs,pbhsd->bshd', w_all.astype(o_all.dtype), o_all).reshape(b, s, -1).astype(hs.dtype)

    ys = jnp.stack([y_a, y_b, y_c, y_d], axis=2)
    gates = jax.nn.sigmoid((z_gate + b_gate).astype(jnp.float32)).reshape(b, s, N_BRANCHES, dm)
    proj = jnp.einsum('bsnc,ncd->bsnd', ys, w_branch)
    merged = jnp.sum(gates.astype(proj.dtype) * proj, axis=2)
    return merged @ w_out


def routed_experts(t, eid, gate, w_gate, w_up, w_down):
    n_tok, dm = t.shape
    n_slots = n_tok * TOP_K
    n_exp = w_gate.shape[0]
    flat_e = eid.reshape(-1).astype(jnp.int32)
    order = jnp.argsort(flat_e)
    sorted_e = flat_e[order]
    counts = jnp.zeros((n_exp,), jnp.int32).at[flat_e].add(1)
    padded = (counts + MOE_BLOCK - 1) // MOE_BLOCK * MOE_BLOCK
    pad_end = jnp.cumsum(padded)
    pad_start = pad_end - padded
    cnt_start = jnp.cumsum(counts) - counts
    dest = pad_start[sorted_e] + jnp.arange(n_slots, dtype=jnp.int32) - cnt_start[sorted_e]
    n_blocks = (n_slots + n_exp * (MOE_BLOCK - 1) + MOE_BLOCK - 1) // MOE_BLOCK
    cap = n_blocks * MOE_BLOCK
    buf_tok = jnp.full((cap,), n_tok, jnp.int32).at[dest].set((order // TOP_K).astype(jnp.int32))
    t_pad = jnp.concatenate([t, jnp.zeros((1, dm), t.dtype)], axis=0)
    xs = t_pad[buf_tok].reshape(n_blocks, MOE_BLOCK, dm)
    block_e = jnp.minimum(jnp.searchsorted(pad_end, jnp.arange(n_blocks, dtype=jnp.int32) * MOE_BLOCK,
                                           side='right'), n_exp - 1)

    def expert_block(args):
        xb, e = args
        return (jax.nn.silu(xb @ w_gate[e]) * (xb @ w_up[e])) @ w_down[e]

    ys = lax.map(expert_block, (xs, block_e)).reshape(cap, dm)
    slot_dest = jnp.zeros((n_slots,), jnp.int32).at[order].set(dest)
    y = ys[slot_dest].reshape(n_tok, TOP_K, dm)
    return jnp.einsum('tk,tkd->td', gate.astype(y.dtype), y)


def hierarchical_moe(hs, rg_w, rg_b, re_w, re_b, w_gate, w_up, w_down):
    b, s, dm = hs.shape
    t = hs.reshape(-1, dm)
    n_tok = t.shape[0]
    tok = jnp.arange(n_tok)
    glog = (t @ rg_w + rg_b).astype(jnp.float32)
    gsel = jnp.argmax(glog, axis=-1)
    pg = jax.nn.softmax(glog, axis=-1)[tok, gsel]
    elog = (t @ re_w + re_b).astype(jnp.float32).reshape(n_tok, N_GROUPS, EXPERTS_PER_GROUP)
    esel = elog[tok, gsel]
    top_v, top_i = lax.top_k(esel, TOP_K)
    gate = jax.nn.softmax(top_v, axis=-1) * pg[:, None]
    eid = gsel[:, None] * EXPERTS_PER_GROUP + top_i
    return routed_experts(t, eid, gate, w_gate, w_up, w_down).reshape(b, s, dm)


def setup_inputs(seed: int = 0) -> dict:
    key = jax.random.key(seed)
    ks = jax.random.split(key, 23)

    def nrm(k, shape, scale):
        return jax.random.normal(k, shape, jnp.float32) * scale

    L = DEPTH
    return {
        'x': nrm(ks[0], (BATCH, SEQ, D_MODEL), 1.0),
        'emb_ln_g': 1.0 + nrm(ks[1], (D_MODEL,), 0.02),
        'emb_ln_b': nrm(ks[2], (D_MODEL,), 0.02),
        'w_in': nrm(ks[3], (L, D_MODEL, IN_COLS), D_MODEL ** -0.5),
        'b_gate': nrm(ks[4], (L, COLS_GATE), 0.02),
        'na_rpb': nrm(ks[5], (L, NA_HEADS, 2 * NA_ROWS - 1, 2 * NA_COLS - 1), 0.1),
        'diff_lam': nrm(ks[6], (L, 4, DIFF_QK_DIM), 0.1),
        'diff_subln_g': 1.0 + nrm(ks[7], (L, DIFF_V_DIM), 0.02),
        'pool_w': nrm(ks[8], (L, len(POOL_WINDOWS), POOL_GROUP, POOL_GROUP), POOL_GROUP ** -0.5),
        'pool_scale': 1.0 + nrm(ks[9], (L, POOL_WIDTH), 0.02),
        'w_branch': nrm(ks[10], (L, N_BRANCHES, BRANCH_WIDTH, D_MODEL), BETA * BRANCH_WIDTH ** -0.5),
        'w_out': nrm(ks[11], (L, D_MODEL, D_MODEL), BETA * D_MODEL ** -0.5),
        'ln1_g': 1.0 + nrm(ks[12], (L, D_MODEL), 0.02),
        'ln1_b': nrm(ks[13], (L, D_MODEL), 0.02),
        'router_group_w': nrm(ks[14], (L, D_MODEL, N_GROUPS), D_MODEL ** -0.5),
        'router_group_b': nrm(ks[15], (L, N_GROUPS), 0.01),
        'router_expert_w': nrm(ks[16], (L, D_MODEL, N_EXPERTS), D_MODEL ** -0.5),
        'router_expert_b': nrm(ks[17], (L, N_EXPERTS), 0.01),
        'expert_w_gate': nrm(ks[18], (L, N_EXPERTS, D_MODEL, D_EXPERT), D_MODEL ** -0.5),
        'expert_w_up': nrm(ks[19], (L, N_EXPERTS, D_MODEL, D_EXPERT), BETA * D_MODEL ** -0.5),
        'expert_w_down': nrm(ks[20], (L, N_EXPERTS, D_EXPERT, D_MODEL), BETA * D_EXPERT ** -0.5),
        'ln2_g': 1.0 + nrm(ks[21], (L, D_MODEL), 0.02),
        'ln2_b': nrm(ks[22], (L, D_MODEL), 0.02),
    }


def reference(x, emb_ln_g, emb_ln_b, w_in, b_gate, na_rpb, diff_lam, diff_subln_g, pool_w, pool_scale,
              w_branch, w_out, ln1_g, ln1_b, router_group_w, router_group_b, router_expert_w,
              router_expert_b, expert_w_gate, expert_w_up, expert_w_down, ln2_g, ln2_b):
    pos = jnp.arange(x.shape[1], dtype=jnp.int32)
    h = layer_norm(x, emb_ln_g, emb_ln_b)
    for l in range(DEPTH):
        lam_init = 0.8 - 0.6 * math.exp(-0.3 * l)
        mix = token_mixing(h, pos, lam_init, w_in[l], b_gate[l], na_rpb[l], diff_lam[l], diff_subln_g[l],
                           pool_w[l], pool_scale[l], w_branch[l], w_out[l])
        h = layer_norm(ALPHA * h + mix, ln1_g[l], ln1_b[l])
        ffn = hierarchical_moe(h, router_group_w[l], router_group_b[l], router_expert_w[l], router_expert_b[l],
                               expert_w_gate[l], expert_w_up[l], expert_w_down[l])
        h = layer_norm(ALPHA * h + ffn, ln2_g[l], ln2_b[l])
    return h
```

```python
import numpy as np
from contextlib import ExitStack
import concourse.bass as bass
import concourse.mybir as mybir
from concourse.bass_utils import run_bass_kernel_spmd

F32 = mybir.dt.float32
BF16 = mybir.dt.bfloat16
I32 = mybir.dt.int32
AF = mybir.ActivationFunctionType
ALU = mybir.AluOpType
AX = mybir.AxisListType

ENGS = ("pe", "act", "dve", "pool", "sp")
SEM_CHUNK = 50000
SAME_ENGINE_WAIT = True


class Buf:
    __slots__ = ("name", "w", "r", "dsem", "dcnt")

    def __init__(self, name=""):
        self.name = name
        self.w = None
        self.r = {}
        self.dsem = None
        self.dcnt = 0


class Prog:
    def __init__(self, nc, es):
        self.nc = nc
        self.es = es
        self.ops = {e: [] for e in ENGS}
        self.seq = {e: 0 for e in ENGS}
        self.known = {e: {} for e in ENGS}
        self.needed = {e: set() for e in ENGS}
        self.nsem = 0
        self.dma_slots = []

    def new_sem(self, name):
        self.nsem += 1
        return self.es.enter_context(self.nc.semaphore(name))

    def _deps(self, eng, reads, writes):
        waits = []
        kn = self.known[eng]

        def need(ev):
            if ev is None:
                return
            if ev[0] == "E":
                src, n = ev[1], ev[2]
                if src == eng and (eng == "pe" or not SAME_ENGINE_WAIT):
                    return
                if kn.get(src, -1) >= n:
                    return
                kn[src] = n
                self.needed[src].add(n)
                waits.append(ev)
            else:
                key = ev[3]
                if kn.get(key, -1) >= ev[2]:
                    return
                kn[key] = ev[2]
                waits.append(ev)

        for b in reads:
            need(b.w)
        for b in writes:
            need(b.w)
            for ev in b.r.values():
                need(ev)
        return waits

    @staticmethod
    def _mark(reads, writes, ev):
        key = ev[1] if ev[0] == "E" else ev[3]
        for b in reads:
            b.r[key] = ev
        for b in writes:
            b.w = ev
            b.r = {}

    def op(self, eng, fn, reads=(), writes=()):
        waits = self._deps(eng, reads, writes)
        n = self.seq[eng]
        self.seq[eng] = n + 1
        self._mark(reads, writes, ("E", eng, n))
        self.ops[eng].append(("op", waits, fn, n))

    def dma(self, q, fn, slot, reads=(), writes=()):
        waits = self._deps(q, reads, writes)
        if slot.dsem is None:
            slot.dsem = self.new_sem("d%d" % self.nsem)
            self.dma_slots.append(slot)
        slot.dcnt += 16
        self._mark(reads, writes, ("D", slot.dsem, slot.dcnt, "d%d" % id(slot)))
        self.ops[q].append(("dma", waits, fn, (slot.dsem, 16)))

    def barrier(self):
        last = {e: self.seq[e] - 1 for e in ("pe", "act", "dve", "pool")}
        for eng in ENGS:
            waits = []
            kn = self.known[eng]
            for src, n in last.items():
                if n < 0 or src == eng:
                    continue
                if kn.get(src, -1) >= n:
                    continue
                kn[src] = n
                self.needed[src].add(n)
                waits.append(("E", src, n))
            for b in self.dma_slots:
                key = "d%d" % id(b)
                if kn.get(key, -1) >= b.dcnt:
                    continue
                kn[key] = b.dcnt
                waits.append(("D", b.dsem, b.dcnt, key))
            self.ops[eng].append(("fence", waits, None, None))

    def wait_all(self, eng, bufs):
        waits = self._deps(eng, bufs, ())
        self.ops[eng].append(("fence", waits, None, None))

    def emit(self):
        rank = {e: {n: i + 1 for i, n in enumerate(sorted(self.needed[e]))} for e in ENGS}
        esems = {}
        for e in ENGS:
            nch = (len(rank[e]) + SEM_CHUNK - 1) // SEM_CHUNK
            esems[e] = [self.new_sem("e_%s_%d" % (e, i)) for i in range(nch)]

        def esem(e, r):
            c = (r - 1) // SEM_CHUNK
            return esems[e][c], r - c * SEM_CHUNK

        ops = self.ops

        def mk(e):
            def body(engine):
                for kind, waits, fn, x in ops[e]:
                    for w in waits:
                        if w[0] == "E":
                            s, v = esem(w[1], rank[w[1]][w[2]])
                        else:
                            s, v = w[1], w[2]
                        engine.wait_ge(s, v)
                    if kind == "fence":
                        continue
                    ins = fn(engine)
                    if kind == "op":
                        if x in rank[e]:
                            s, v = esem(e, rank[e][x])
                            ins.then_inc(s, 1)
                    else:
                        ins.then_inc(x[0], x[1])
            return body

        with self.nc.Block() as block:
            block.tensor(mk("pe"))
            block.scalar(mk("act"))
            block.vector(mk("dve"))
            block.gpsimd(mk("pool"))
            block.sync(mk("sp"))


class Arena:
    def __init__(self, ap_f32, nwords):
        self.ap = ap_f32
        self.n = nwords
        self.off = 0
        self.limit = nwords

    def top_view(self, word_off_from_top, free_elems, dtype=F32):
        words = free_elems if dtype == F32 or dtype == I32 else (free_elems + 1) // 2
        a = self.n - word_off_from_top
        v = self.ap[:, a:a + words]
        if dtype != F32:
            v = v.bitcast(dtype)
        return v[:, 0:free_elems]

    def mark(self):
        return self.off

    def release(self, m):
        self.off = m

    def alloc(self, free_elems, dtype=F32, parts=128):
        words = free_elems if dtype == F32 or dtype == I32 else (free_elems + 1) // 2
        words = (words + 7) // 8 * 8
        assert self.off + words <= self.limit, ("SBUF arena overflow", self.off, words, self.limit)
        v = self.ap[0:parts, self.off:self.off + words]
        self.off += words
        if dtype != F32:
            v = v.bitcast(dtype)
        return v[:, 0:free_elems]


def mm(P, out, lhsT, rhs, start=True, stop=True, reads=(), writes=()):
    P.op("pe", lambda e: e.matmul(out, lhsT, rhs, start=start, stop=stop), reads=reads, writes=writes)


def trp(P, out, in_, ident, reads=(), writes=()):
    P.op("pe", lambda e: e.transpose(out, in_, ident), reads=reads, writes=writes)


def act(P, out, in_, func, bias=None, scale=None, reads=(), writes=()):
    kw = {}
    if bias is not None:
        kw["bias"] = bias
    if scale is not None:
        kw["scale"] = scale
    P.op("act", lambda e: e.activation(out, in_, func, **kw), reads=reads, writes=writes)


def cp(P, eng, out, in_, reads=(), writes=()):
    if eng == "act":
        P.op("act", lambda e: e.copy(out, in_), reads=reads, writes=writes)
    else:
        P.op(eng, lambda e: e.tensor_copy(out, in_), reads=reads, writes=writes)


def tt(P, eng, out, in0, in1, op, reads=(), writes=()):
    P.op(eng, lambda e: e.tensor_tensor(out, in0, in1, op), reads=reads, writes=writes)


def ts(P, eng, out, in0, s1, s2, op0, op1=None, reads=(), writes=()):
    if op1 is None:
        P.op(eng, lambda e: e.tensor_scalar(out, in0, s1, None, op0), reads=reads, writes=writes)
    else:
        P.op(eng, lambda e: e.tensor_scalar(out, in0, s1, s2, op0, op1), reads=reads, writes=writes)


def stt(P, eng, out, in0, scalar, in1, op0, op1, reads=(), writes=()):
    P.op(eng, lambda e: e.scalar_tensor_tensor(out, in0, scalar, in1, op0, op1), reads=reads, writes=writes)


def recip(P, out, in_, reads=(), writes=()):
    P.op("dve", lambda e: e.reciprocal(out, in_), reads=reads, writes=writes)


def mset(P, eng, ap, val, writes=()):
    P.op(eng, lambda e: e.memset(ap, val), writes=writes)


def dma(P, q, out, in_, slot, reads=(), writes=()):
    P.dma(q, lambda e: e.dma_start(out=out, in_=in_), slot, reads=reads, writes=writes)


import math
import numpy as np

D = 1024
S = 4096
OWN = 2048
LN_EPS = 1e-5
ROPE_THETA = 500000.0
ALPHA = 4 ** 0.25
NCH_T = 72
COL_V = NCH_T * 128
NCOL = COL_V + 6 * 256 + 128
T_NA_Q, T_NA_K = 0, 2
T_DF_Q, T_DF_QP, T_DF_K, T_DF_KP = 4, 7, 10, 13
T_DL = 16
T_GATE = 40
V_NA, V_DF, V_DL, V_POOL = 0, 1, 2, 5


def w_in_columns():
    cols = np.full(NCOL, -1, np.int64)
    def put(chunk, arr):
        cols[chunk * 128: chunk * 128 + len(arr)] = arr
    put(T_NA_Q, np.arange(0, 256)); put(T_NA_K, np.arange(256, 512))
    for which, (tb, tp) in enumerate(((T_DF_Q, T_DF_QP), (T_DF_K, T_DF_KP))):
        base = 768 + which * 256
        for blk in range(8):
            d = np.arange(32)
            orig = base + blk * 32 + d
            part = base + blk * 32 + np.where(d < 4, d + 4, np.where(d < 8, d - 4, d))
            c, o = divmod(blk, 3)
            cols[(tb + c) * 128 + o * 32:(tb + c) * 128 + o * 32 + 32] = orig
            cols[(tp + c) * 128 + o * 32:(tp + c) * 128 + o * 32 + 32] = part
    for p in range(3):
        for which in range(2):
            for h in range(4):
                d = np.arange(64)
                base = 1792 + ((p * 3 + which) * 4 + h) * 64
                orig = base + d
                part = base + np.where(d < 8, d + 8, np.where(d < 16, d - 8, d))
                tb = T_DL + p * 8 + which * 4
                cols[tb * 128 + h * 64: tb * 128 + h * 64 + 64] = orig
                cols[(tb + 2) * 128 + h * 64:(tb + 2) * 128 + h * 64 + 64] = part
    put(T_GATE, np.arange(4096, 8192))
    v0 = COL_V
    cols[v0:v0 + 256] = np.arange(512, 768)
    cols[v0 + 256:v0 + 512] = 768 + 512 + np.arange(256)
    for p in range(3):
        cols[v0 + 512 + p * 256: v0 + 768 + p * 256] = 1792 + ((p * 3 + 2) * 4) * 64 + np.arange(256)
    cols[v0 + 1280:v0 + 1536] = 1536 + np.arange(256)
    return cols


def relayout_w_in(w_in_l):
    cols = w_in_columns()
    out = np.zeros((D, NCOL), np.float32)
    m = cols >= 0
    out[:, m] = w_in_l[:, cols[m]]
    return out


def local_pos(hf):
    t = np.arange(S)
    return t if hf == 0 else (S - 1 - t)


def rope_tables(hf, rot, period):
    half = rot // 2
    pos = local_pos(hf).astype(np.float32)
    inv = np.exp(np.arange(half, dtype=np.float32) * np.float32(-2.0 * math.log(ROPE_THETA) / rot)).astype(np.float32)
    ang = (pos[:, None] * inv[None, :]).astype(np.float32)
    cos, sin = np.cos(ang).astype(np.float32), np.sin(ang).astype(np.float32)
    Ct = np.ones((128, S), np.float32); St = np.zeros((128, S), np.float32)
    for row in range(128):
        d = row % period
        if d < half:
            Ct[row] = cos[:, d]; St[row] = -sin[:, d]
        elif d < rot:
            Ct[row] = cos[:, d - half]; St[row] = sin[:, d - half]
    return np.stack([Ct, St])


def na_tables(hf, rpb):
    out = np.empty((128, 4, 3, 640), np.float32)
    kk = np.arange(128); qq = np.arange(128)
    for s in range(3):
        j = s
        kb = max(2 * j - 4, 0)
        for c in range(5):
            kr_l = kb + 2 * c + kk // 64; ck_l = kk % 64
            qr_l = 2 * j + qq // 64; cq_l = qq % 64
            if hf == 0:
                kr, ck, qr, cq = kr_l, ck_l, qr_l, cq_l
            else:
                kr, ck, qr, cq = 63 - kr_l, 63 - ck_l, 63 - qr_l, 63 - cq_l
            ws = np.clip(qr - 4, 0, 56); cs = np.clip(cq - 8, 0, 48)
            valid = ((kr[:, None] >= ws[None, :]) & (kr[:, None] < ws[None, :] + 8) &
                     (ck[:, None] >= cs[None, :]) & (ck[:, None] < cs[None, :] + 16))
            dr = np.clip(kr[:, None] - qr[None, :] + 7, 0, 14)
            dc = np.clip(ck[:, None] - cq[None, :] + 15, 0, 30)
            for h in range(4):
                out[:, h, s, c * 128:(c + 1) * 128] = np.where(valid, rpb[h][dr, dc], np.float32(-30000.0))
    return out.reshape(128, 4 * 3 * 640)


class Ctx:
    pass


def bank(P, i):
    return P.psum[i], P.psb[i]


def stage_ln(P, A, C, do_ln):
    m = A.mark()
    if do_ln:
        gB = A.alloc(1024); bB = A.alloc(1024)
        b_gB, b_bB = Buf("gB"), Buf("bB")
        dma(P, "sp", gB, C.lng.partition_broadcast(128), b_gB, writes=[b_gB])
        dma(P, "sp", bB, C.lnb.partition_broadcast(128), b_bB, writes=[b_bB])
    NX = 3
    xs = [A.alloc(1024) for _ in range(NX)]; b_xs = [Buf("xs%d" % i) for i in range(NX)]
    hs = [A.alloc(1024) for _ in range(2)]; b_hs = [Buf("hs%d" % i) for i in range(2)]
    st = [A.alloc(16) for _ in range(2)]; b_st = [Buf("st%d" % i) for i in range(2)]
    for t in range(32):
        x = xs[t % NX]; bx = b_xs[t % NX]
        h = hs[t % 2]; bh = b_hs[t % 2]
        s = st[t % 2]; bs = b_st[t % 2]
        dma(P, "sp", x, C.xin[t * 128:(t + 1) * 128, :], bx, writes=[bx])
        if do_ln:
            stats = s[:, 0:12]; mv = s[:, 12:14]; rstd = s[:, 14:15]; nmr = s[:, 15:16]
            P.op("dve", lambda e, x=x, stats=stats: e.bn_stats(stats[:, 0:6], x[:, 0:512]), reads=[bx], writes=[bs])
            P.op("dve", lambda e, x=x, stats=stats: e.bn_stats(stats[:, 6:12], x[:, 512:1024]), reads=[bx], writes=[bs])
            P.op("dve", lambda e, stats=stats, mv=mv: e.bn_aggr(mv, stats.rearrange("p (c s) -> p c s", s=6)),
                 reads=[bs], writes=[bs])
            act(P, rstd, mv[:, 1:2], AF.Sqrt, bias=C.eps, scale=1.0, reads=[bs, C.eps_b], writes=[bs])
            recip(P, rstd, rstd, reads=[bs], writes=[bs])
            stt(P, "dve", nmr, mv[:, 0:1], -1.0, rstd, ALU.mult, ALU.mult, reads=[bs], writes=[bs])
            act(P, h, x, AF.Identity, bias=nmr, scale=rstd, reads=[bx, bs], writes=[bh])
            tt(P, "pool", h, h, gB, ALU.mult, reads=[bh, b_gB], writes=[bh])
            tt(P, "dve", h, h, bB, ALU.add, reads=[bh, b_bB], writes=[bh])
            src = h; bsrc = bh
        else:
            src = x; bsrc = bx
        if t < 16:
            dma(P, "sp", C.hres[t * 128:(t + 1) * 128, :], src, bsrc, reads=[bsrc], writes=[C.hres_b[t]])
        for half in range(2):
            pb, bpb = bank(P, C.rot % 8); C.rot += 1
            for j in range(4):
                kc = half * 4 + j
                trp(P, pb[:, j * 128:(j + 1) * 128], src[:, kc * 128:(kc + 1) * 128], C.ident, reads=[bsrc, C.ident_b], writes=[bpb])
            dst = C.hT[:, half * 4:(half + 1) * 4, t * 128:(t + 1) * 128]
            cp(P, "act" if half == 0 else "dve", dst, pb.rearrange("p (j t) -> p j t", j=4), writes=[C.hT_b[t // 4], bpb])
    A.release(m)


def load_w(P, A, C, col0, ncols, name):
    t = A.alloc(8 * ncols, BF16).rearrange("p (k c) -> p k c", k=8)
    b = Buf(name)
    src = C.w_in[:, col0:col0 + ncols].rearrange("(k p) c -> p k c", p=128)
    dma(P, "pool", t, src, b, writes=[b])
    return t, b


def proj_T(P, C, W, bW, wcol, tcs, banks, dst_fn, dst_buf_fn, rope=None, wcol_p=None):
    for tc in tcs:
        ps, bps = bank(P, banks[C.rot % len(banks)]); C.rot += 1
        for k in range(8):
            mm(P, ps, W[:, k, wcol:wcol + 128], C.hT[:, k, tc * 512:(tc + 1) * 512], start=(k == 0), stop=(k == 7),
               reads=[bW, C.hT_b[tc]], writes=[bps])
        dst = dst_fn(tc); bd = dst_buf_fn(tc)
        if rope is None:
            cp(P, "act" if C.alt % 2 == 0 else "dve", dst, ps, writes=[bd, bps]); C.alt += 1
        else:
            ps2, bps2 = bank(P, banks[C.rot % len(banks)]); C.rot += 1
            for k in range(8):
                mm(P, ps2, W[:, k, wcol_p:wcol_p + 128], C.hT[:, k, tc * 512:(tc + 1) * 512], start=(k == 0), stop=(k == 7),
                   reads=[bW, C.hT_b[tc]], writes=[bps2])
            tabC, tabS, btab = rope(tc)
            i = C.rt % 2; C.rt += 1
            t1, t2 = C.rt1[i], C.rt2[i]; b1, b2 = C.rt1_b[i], C.rt2_b[i]
            tt(P, "dve", t1, ps, tabC, ALU.mult, reads=[btab], writes=[b1, bps])
            tt(P, "dve", t2, ps2, tabS, ALU.mult, reads=[btab], writes=[b2, bps2])
            tt(P, "pool", dst, t1, t2, ALU.add, reads=[b1, b2], writes=[bd])


def proj_V(P, C, W, bW, wcol, lhs_list, banks, dst_fn, dst_buf_fn):
    for i, (hb, lf, ntok) in enumerate(lhs_list):
        ps, bps = bank(P, banks[C.rot % len(banks)]); C.rot += 1
        for k in range(8):
            mm(P, ps[0:ntok, 0:256], lf(k), W[:, k, wcol:wcol + 256], start=(k == 0), stop=(k == 7),
               reads=[bW] + hb, writes=[bps])
        dst = dst_fn(i); bd = dst_buf_fn(i)
        src = ps[0:ntok, 0:256].rearrange("p (h d) -> p h d", h=4)
        cp(P, "act" if C.alt % 2 == 0 else "dve", dst, src, writes=[bd, bps]); C.alt += 1


def normalize_out(P, C, O, bO, n, dst, bdst, bcbank):
    i = C.rdi % 2; C.rdi += 1
    rd = C.rd[i]; brd = C.rd_b[i]; bcs = C.bcs[i]; bbcs = C.bcs_b[i]
    bc, bbc = bank(P, bcbank)
    recip(P, rd[64:65, 0:n], O[64:65, 0:n], writes=[brd, bO])
    mm(P, bc[0:64, 0:n], C.ones_f[64:65, 0:64], rd[64:65, 0:n], reads=[brd, C.ones_b], writes=[bbc])
    cp(P, "act", bcs[0:64, 0:n], bc[0:64, 0:n], writes=[bbcs, bbc])
    tt(P, "dve", dst, O[0:64, 0:n], bcs[0:64, 0:n], ALU.mult, reads=[bbcs], writes=[bdst, bO])


def mixer_na(P, A, C):
    m0 = A.mark()
    mixer_scratch(A, C, rope=False)
    Wt, bWt = load_w(P, A, C, T_NA_Q * 128, 512, "w_na_t")
    Wv, bWv = load_w(P, A, C, COL_V + V_NA * 256, 256, "w_na_v")
    QT = A.alloc(2 * 2048, BF16).rearrange("p (c t) -> p c t", c=2); QT_b = [Buf("naQ%d" % i) for i in range(4)]
    KT = A.alloc(2 * 2560, BF16).rearrange("p (c t) -> p c t", c=2); KT_b = [Buf("naK%d" % i) for i in range(5)]
    NVT = 20
    VA = A.alloc(NVT * 4 * 65, BF16).rearrange("p (t h d) -> p t h d", t=NVT, h=4); VA_b = [Buf("naV%d" % i) for i in range(NVT)]
    tab = A.alloc(4 * 3 * 640).rearrange("p (h s x) -> p h s x", h=4, s=3); btab = Buf("natab")
    dma(P, "sp", tab.rearrange("p h s x -> p (h s x)"), C.na_tab, btab, writes=[btab])
    for tl in range(NVT):
        mset(P, "pool", VA[:, tl, :, 64:65], 1.0, writes=[VA_b[tl]])
    T1 = [A.alloc(640) for _ in range(2)]; T1_b = [Buf() for _ in range(2)]
    E = [A.alloc(640, BF16) for _ in range(2)]; E_b = [Buf() for _ in range(2)]
    for cc in range(2):
        proj_T(P, C, Wt, bWt, cc * 128, range(4), [0, 1], lambda tc, cc=cc: QT[:, cc, tc * 512:(tc + 1) * 512], lambda tc: QT_b[tc])
        proj_T(P, C, Wt, bWt, 256 + cc * 128, range(5), [0, 1], lambda tc, cc=cc: KT[:, cc, tc * 512:(tc + 1) * 512], lambda tc: KT_b[tc])
    proj_V(P, C, Wv, bWv, 0,
           [([C.hT_b[tl // 4]], (lambda k, tl=tl: C.hT[:, k, tl * 128:(tl + 1) * 128]), 128) for tl in range(NVT)],
           [0, 1], lambda i: VA[:, i, :, 0:64], lambda i: VA_b[i])
    ssets = [(2, 3), (6, 7)]
    it = 0
    for h in range(4):
        ch, pr = divmod(h, 2)
        for G in range(4):
            O, bO = bank(P, 4 + (C.oi % 2)); C.oi += 1
            for jj in range(4):
                j = 4 * G + jj
                s = min(j, 2); kb = max(2 * j - 4, 0)
                (sa, sb) = ssets[it % 2]
                Sa, bSa = bank(P, sa); Sb, bSb = bank(P, sb)
                t1 = T1[it % 2]; bt1 = T1_b[it % 2]; ee = E[it % 2]; bee = E_b[it % 2]
                it += 1
                for c in range(5):
                    tok0 = (kb + 2 * c) * 64
                    out = Sa[:, c * 128:(c + 1) * 128] if c < 4 else Sb[:, 0:128]
                    bout = bSa if c < 4 else bSb
                    mm(P, out, KT[pr * 64:(pr + 1) * 64, ch, tok0:tok0 + 128], QT[pr * 64:(pr + 1) * 64, ch, j * 128:(j + 1) * 128],
                       reads=[KT_b[tok0 // 512], KT_b[(tok0 + 127) // 512], QT_b[j // 4]], writes=[bout])
                stt(P, "dve", t1[:, 0:512], Sa, 0.125, tab[:, h, s, 0:512], ALU.mult, ALU.add, reads=[btab], writes=[bt1, bSa])
                stt(P, "dve", t1[:, 512:640], Sb[:, 0:128], 0.125, tab[:, h, s, 512:640], ALU.mult, ALU.add, reads=[btab], writes=[bt1, bSb])
                act(P, ee, t1, AF.Exp, reads=[bt1], writes=[bee])
                for c in range(5):
                    vt = kb // 2 + c
                    mm(P, O[0:65, jj * 128:(jj + 1) * 128], VA[:, vt, h, 0:65], ee[:, c * 128:(c + 1) * 128],
                       start=(c == 0), stop=(c == 4), reads=[VA_b[vt], bee], writes=[bO])
            dst = C.yT[pr * 64:(pr + 1) * 64, 0 * 2 + ch, G * 512:(G + 1) * 512]
            normalize_out(P, C, O, bO, 512, dst, C.yT_b[0][G], 0)
    A.release(m0)


def setup_common(P, A, C, nc):
    C.rot = 0; C.alt = 0; C.rt = 0; C.oi = 0; C.rdi = 0
    C.eps = A.alloc(1); C.eps_b = Buf("eps")
    mset(P, "pool", C.eps, LN_EPS, writes=[C.eps_b])
    C.ident = A.alloc(128); C.ident_b = Buf("ident")
    dma(P, "sp", C.ident, C.ident_d, C.ident_b, writes=[C.ident_b])
    C.ones_f = A.alloc(128); C.ones_b = Buf("ones")
    mset(P, "pool", C.ones_f, 1.0, writes=[C.ones_b])


def mixer_scratch(A, C, rope=True):
    C.rd = [A.alloc(512) for _ in range(2)]; C.rd_b = [Buf() for _ in range(2)]
    C.bcs = [A.alloc(512) for _ in range(2)]; C.bcs_b = [Buf() for _ in range(2)]
    if rope:
        C.rt1 = [A.alloc(512) for _ in range(2)]; C.rt1_b = [Buf() for _ in range(2)]
        C.rt2 = [A.alloc(512) for _ in range(2)]; C.rt2_b = [Buf() for _ in range(2)]


def rope_loader(P, A, C, dram_tab):
    slots = [A.alloc(1024).rearrange("p (a t) -> p a t", a=2) for _ in range(2)]
    bufs = [Buf("rope%d" % i) for i in range(2)]
    state = {"tc": [None, None], "n": 0}

    def rope(tc):
        for i in range(2):
            if state["tc"][i] == tc:
                return slots[i][:, 0, :], slots[i][:, 1, :], bufs[i]
        i = state["n"] % 2; state["n"] += 1
        state["tc"][i] = tc
        dma(P, "sp", slots[i], dram_tab[:, :, tc * 512:(tc + 1) * 512].rearrange("a p t -> p a t"), bufs[i], writes=[bufs[i]])
        return slots[i][:, 0, :], slots[i][:, 1, :], bufs[i]
    return rope


def mixer_diff(P, A, C, lam_init):
    m0 = A.mark()
    mixer_scratch(A, C)
    QT = A.alloc(3 * 2048, BF16).rearrange("p (c t) -> p c t", c=3); QT_b = [Buf("dfQ%d" % i) for i in range(4)]
    KT = A.alloc(3 * 4096, BF16).rearrange("p (c t) -> p c t", c=3); KT_b = [Buf("dfK%d" % i) for i in range(8)]
    NVT = 32
    VA = A.alloc(NVT * 4 * 65, BF16).rearrange("p (t h d) -> p t h d", t=NVT, h=4); VA_b = [Buf("dfV%d" % i) for i in range(NVT)]
    for tl in range(NVT):
        mset(P, "pool", VA[:, tl, :, 64:65], 1.0, writes=[VA_b[tl]])
    dl = A.alloc(128); bdl = Buf("dl")
    dma(P, "sp", dl, C.diff_lam.partition_broadcast(128), bdl, writes=[bdl])
    sc = A.alloc(16); bsc = Buf("dfsc")
    dlv = dl.rearrange("p (a b) -> p a b", a=4)
    pr2 = A.alloc(64).rearrange("p (a b) -> p a b", a=2); bpr2 = Buf()
    tt(P, "dve", pr2, dlv[:, 0:4:2, :], dlv[:, 1:4:2, :], ALU.mult, reads=[bdl], writes=[bpr2])
    P.op("dve", lambda e: e.reduce_sum(sc[:, 0:2], pr2, axis=AX.X), reads=[bpr2], writes=[bsc])
    act(P, sc[:, 2:4], sc[:, 0:2], AF.Exp, reads=[bsc], writes=[bsc])
    tt(P, "dve", sc[:, 4:5], sc[:, 2:3], sc[:, 3:4], ALU.subtract, reads=[bsc], writes=[bsc])
    ts(P, "dve", sc[:, 5:6], sc[:, 4:5], float(lam_init), -1.0, ALU.add, ALU.mult, reads=[bsc], writes=[bsc])
    nlam = sc[:, 5:6]
    gs = A.alloc(1); bgs = Buf("gs")
    gsrc = C.diff_g.rearrange("a (p o) -> (a p) o", o=1)
    dma(P, "sp", gs[0:64, :], gsrc, bgs, writes=[bgs])
    dma(P, "sp", gs[64:128, :], gsrc, bgs, writes=[bgs])
    ts(P, "dve", gs, gs, float(1.0 - lam_init), None, ALU.mult, reads=[bgs], writes=[bgs])
    od = A.alloc(64); bod = Buf("onesdiv")
    mset(P, "pool", od, 1.0 / 64.0, writes=[bod])
    m1 = A.mark()
    rope = rope_loader(P, A, C, C.rope_d)
    Wv, bWv = load_w(P, A, C, COL_V + V_DF * 256, 256, "w_df_v")
    for part in range(2):
        m2 = A.mark()
        Wt, bWt = load_w(P, A, C, (T_DF_Q + part * 6) * 128, 768, "w_df_t%d" % part)
        for tc in range(4 if part == 0 else 8):
            for cc in range(3):
                if part == 0:
                    proj_T(P, C, Wt, bWt, cc * 128, [tc], [0, 1, 2, 3], lambda tc, cc=cc: QT[:, cc, tc * 512:(tc + 1) * 512],
                           lambda tc: QT_b[tc], rope=rope, wcol_p=384 + cc * 128)
                else:
                    proj_T(P, C, Wt, bWt, cc * 128, [tc], [0, 1, 2, 3], lambda tc, cc=cc: KT[:, cc, tc * 512:(tc + 1) * 512],
                           lambda tc: KT_b[tc], rope=rope, wcol_p=384 + cc * 128)
        P.barrier()
        A.release(m2)
    proj_V(P, C, Wv, bWv, 0,
           [([C.hT_b[tl // 4]], (lambda k, tl=tl: C.hT[:, k, tl * 128:(tl + 1) * 128]), 128) for tl in range(NVT)],
           [0, 1, 2, 3], lambda i: VA[:, i, :, 0:64], lambda i: VA_b[i])
    P.barrier()
    A.release(m1)
    NE = 4
    E = [A.alloc(512, BF16) for _ in range(NE)]; E_b = [Buf() for _ in range(NE)]
    o0 = A.alloc(512); o1 = A.alloc(512); sq = A.alloc(512); rr = A.alloc(512)
    bo0, bo1, bsq, brr = Buf(), Buf(), Buf(), Buf()
    rd2 = A.alloc(512); brd2 = Buf()
    bcs1 = A.alloc(512); bbcs1 = Buf()
    scale = 32 ** -0.5
    ei = 0; si = 0; ui = 0
    FB = 3
    for h in range(4):
        ch, pr = divmod(h, 2)
        for qc in range(4):
            ub = (4, 5) if ui % 2 == 0 else (6, 7); ui += 1
            U = [bank(P, ub[0]), bank(P, ub[1])]
            for m in range(2):
                blk = h * 2 + m; cch, o = divmod(blk, 3); base = o * 32
                Um, bUm = U[m]
                for kc in range(32):
                    Sps, bS = bank(P, si % 3); si += 1
                    ee = E[ei % NE]; bee = E_b[ei % NE]; ei += 1
                    mm(P, Sps, KT[base:base + 32, cch, kc * 128:(kc + 1) * 128], QT[base:base + 32, cch, qc * 512:(qc + 1) * 512],
                       reads=[KT_b[kc // 4], QT_b[qc]], writes=[bS])
                    act(P, ee, Sps, AF.Exp, scale=scale, writes=[bee, bS])
                    mm(P, Um[0:65, :], VA[:, kc, h, 0:65], ee, start=(kc == 0), stop=(kc == 31), reads=[VA_b[kc], bee], writes=[bUm])
            (U0, bU0), (U1, bU1) = U
            i = C.rdi % 2; C.rdi += 1
            rd = C.rd[i]; brd = C.rd_b[i]; bcs = C.bcs[i]; bbcs = C.bcs_b[i]
            bc, bbc = bank(P, FB)
            recip(P, rd[64:65, :], U0[64:65, :], writes=[brd, bU0])
            recip(P, rd2[64:65, :], U1[64:65, :], writes=[brd2, bU1])
            ts(P, "dve", rd2[64:65, :], rd2[64:65, :], nlam[64:65, :], None, ALU.mult, reads=[bsc], writes=[brd2])
            mm(P, bc[0:64, :], C.ones_f[64:65, 0:64], rd[64:65, :], reads=[brd, C.ones_b], writes=[bbc])
            cp(P, "act", bcs[0:64, :], bc[0:64, :], writes=[bbcs, bbc])
            tt(P, "dve", o0[0:64, :], U0[0:64, :], bcs[0:64, :], ALU.mult, reads=[bbcs], writes=[bo0, bU0])
            mm(P, bc[0:64, :], C.ones_f[64:65, 0:64], rd2[64:65, :], reads=[brd2, C.ones_b], writes=[bbc])
            cp(P, "act", bcs1[0:64, :], bc[0:64, :], writes=[bbcs1, bbc])
            tt(P, "dve", o1[0:64, :], U1[0:64, :], bcs1[0:64, :], ALU.mult, reads=[bbcs1], writes=[bo1, bU1])
            tt(P, "pool", o0[0:64, :], o0[0:64, :], o1[0:64, :], ALU.add, reads=[bo1], writes=[bo0])
            tt(P, "pool", sq[0:64, :], o0[0:64, :], o0[0:64, :], ALU.mult, reads=[bo0], writes=[bsq])
            mm(P, bc[0:64, :], od[0:64, 0:64], sq[0:64, :], reads=[bsq, bod], writes=[bbc])
            act(P, rr[0:64, :], bc[0:64, :], AF.Sqrt, bias=C.eps[0:64, :], scale=1.0, reads=[C.eps_b], writes=[brr, bbc])
            recip(P, rr[0:64, :], rr[0:64, :], writes=[brr])
            dst = C.yT[pr * 64:(pr + 1) * 64, 2 + ch, qc * 512:(qc + 1) * 512]
            stt(P, "dve", dst, o0[0:64, :], gs[0:64, 0:1], rr[0:64, :], ALU.mult, ALU.mult,
                reads=[bo0, brr, bgs], writes=[C.yT_b[1][qc]])
    P.barrier()
    A.release(m0)


def mixer_dil(P, A, C):
    m0 = A.mark()
    acc = [A.alloc(2048) for _ in range(4)]; acc_b = [[Buf("acc%d_%d" % (h, q)) for q in range(4)] for h in range(4)]
    msk = A.alloc(3 * 128, BF16).rearrange("p (a t) -> p a t", a=3); bmsk = Buf("dmask")
    dma(P, "pool", msk, C.dmask.rearrange("p (a t) -> p a t", a=3), bmsk, writes=[bmsk])
    for p, r in enumerate((1, 4, 16)):
        m1 = A.mark()
        nb = 16 // r
        ntc = (5, 5, 6)[p]
        QT = A.alloc(2 * 2048, BF16).rearrange("p (c t) -> p c t", c=2); QT_b = [Buf("dlQ%d" % i) for i in range(4)]
        KT = A.alloc(2 * ntc * 512, BF16).rearrange("p (c t) -> p c t", c=2); KT_b = [Buf("dlK%d" % i) for i in range(ntc)]
        NVT = r * (nb + 1)
        VA = A.alloc(NVT * 4 * 65, BF16).rearrange("p (t h d) -> p t h d", t=NVT, h=4); VA_b = [Buf("dlV%d" % i) for i in range(NVT)]
        for tl in range(NVT):
            mset(P, "pool", VA[:, tl, :, 64:65], 1.0, writes=[VA_b[tl]])
        m2 = A.mark()
        C.rt1 = [A.alloc(512) for _ in range(2)]; C.rt1_b = [Buf() for _ in range(2)]
        C.rt2 = [A.alloc(512) for _ in range(2)]; C.rt2_b = [Buf() for _ in range(2)]
        rope = rope_loader(P, A, C, C.rope_l)
        Wv, bWv = load_w(P, A, C, COL_V + (V_DL + p) * 256, 256, "w_dl_v%d" % p)
        for part in range(2):
            m3 = A.mark()
            Wt, bWt = load_w(P, A, C, (T_DL + p * 8 + part * 4) * 128, 512, "w_dl_t%d_%d" % (p, part))
            for tc in range(4 if part == 0 else ntc):
                for cc in range(2):
                    if part == 0:
                        proj_T(P, C, Wt, bWt, cc * 128, [tc], [0, 1, 2, 3], lambda tc, cc=cc: QT[:, cc, tc * 512:(tc + 1) * 512],
                               lambda tc: QT_b[tc], rope=rope, wcol_p=256 + cc * 128)
                    else:
                        proj_T(P, C, Wt, bWt, cc * 128, [tc], [0, 1, 2, 3], lambda tc, cc=cc: KT[:, cc, tc * 512:(tc + 1) * 512],
                               lambda tc: KT_b[tc], rope=rope, wcol_p=256 + cc * 128)
            P.barrier()
            A.release(m3)
        lhs_list = []
        for m in range(r):
            for c in range(nb + 1):
                if c == 0:
                    start, ntok = m, 64
                else:
                    start, ntok = (128 * c - 64) * r + m, 128
                last = start + (ntok - 1) * r
                hb = [C.hT_b[i] for i in range(start // 512, last // 512 + 1)]
                lhs_list.append((hb, (lambda k, start=start, ntok=ntok: C.hT[:, k, start:start + (ntok - 1) * r + 1:r]), ntok))
        proj_V(P, C, Wv, bWv, 0, lhs_list, [0, 1, 2, 3],
               lambda i: VA[0:lhs_list[i][2], i, :, 0:64], lambda i: VA_b[i])
        P.barrier()
        A.release(m2)
        NE = 3
        E = [A.alloc(256, BF16) for _ in range(NE)]; E_b = [Buf() for _ in range(NE)]
        Em = [A.alloc(256, BF16) for _ in range(NE)]; Em_b = [Buf() for _ in range(NE)]
        ei = 0; si = 0
        for h in range(4):
            ch, pr = divmod(h, 2)
            rows = slice(pr * 64, (pr + 1) * 64)
            for m in range(r):
                for n in range(nb):
                    q0 = 128 * n * r + m
                    qap = QT[rows, ch, q0:q0 + 127 * r + 1:r]
                    Sps, bS = bank(P, si % 4); Ups, bU = bank(P, 4 + si % 4); si += 1
                    ee = E[ei % NE]; bee = E_b[ei % NE]; em = Em[ei % NE]; bem = Em_b[ei % NE]; ei += 1
                    if n == 0:
                        ka, nk = m, 64
                    else:
                        ka, nk = (128 * n - 64) * r + m, 128
                    kbs = (128 * n + 64) * r + m
                    lastA = ka + (nk - 1) * r; lastB = kbs + 127 * r
                    mm(P, Sps[0:nk, 0:128], KT[rows, ch, ka:lastA + 1:r], qap,
                       reads=[KT_b[i] for i in range(ka // 512, lastA // 512 + 1)] + [QT_b[q0 // 512], QT_b[(q0 + 127 * r) // 512]], writes=[bS])
                    mm(P, Sps[:, 128:256], KT[rows, ch, kbs:lastB + 1:r], qap,
                       reads=[KT_b[i] for i in range(kbs // 512, lastB // 512 + 1)], writes=[bS])
                    if n == 0:
                        act(P, ee[0:64, 0:128], Sps[0:64, 0:128], AF.Exp, scale=0.125, writes=[bee, bS])
                        act(P, ee[:, 128:256], Sps[:, 128:256], AF.Exp, scale=0.125, writes=[bee, bS])
                        tt(P, "pool", em[0:64, 0:128], ee[0:64, 0:128], msk[0:64, 2, :], ALU.mult, reads=[bee, bmsk], writes=[bem])
                        tt(P, "pool", em[:, 128:256], ee[:, 128:256], msk[:, 1, :], ALU.mult, reads=[bee, bmsk], writes=[bem])
                    else:
                        act(P, ee, Sps[:, 0:256], AF.Exp, scale=0.125, writes=[bee, bS])
                        tt(P, "pool", em.rearrange("p (a t) -> p a t", a=2), ee.rearrange("p (a t) -> p a t", a=2), msk[:, 0:2, :], ALU.mult,
                           reads=[bee, bmsk], writes=[bem])
                    ta = m * (nb + 1) + n
                    mm(P, Ups[0:65, 0:128], VA[0:nk, ta, h, 0:65], em[0:nk, 0:128], start=True, stop=False, reads=[VA_b[ta], bem], writes=[bU])
                    mm(P, Ups[0:65, 0:128], VA[:, ta + 1, h, 0:65], em[:, 128:256], start=False, stop=True, reads=[VA_b[ta + 1], bem], writes=[bU])
                    dst = acc[h][0:65, q0:q0 + 127 * r + 1:r]
                    ab = list({acc_b[h][q0 // 512], acc_b[h][(q0 + 127 * r) // 512]})
                    if p == 0:
                        cp(P, "dve", dst, Ups[0:65, 0:128], writes=ab + [bU])
                    else:
                        tt(P, "dve", dst, dst, Ups[0:65, 0:128], ALU.add, writes=ab + [bU])
        P.barrier()
        A.release(m1)
    mixer_scratch(A, C, rope=False)
    for h in range(4):
        ch, pr = divmod(h, 2)
        for qc in range(4):
            dst = C.yT[pr * 64:(pr + 1) * 64, 6 + ch, qc * 512:(qc + 1) * 512]
            normalize_out(P, C, acc[h][:, qc * 512:(qc + 1) * 512], acc_b[h][qc], 512, dst, C.yT_b[3][qc], 0)
    P.barrier()
    A.release(m0)


def mixer_pool(P, A, C):
    m0 = A.mark()
    Wv, bWv = load_w(P, A, C, COL_V + V_POOL * 256, 256, "w_pool")
    NT = 17
    U32 = A.alloc(NT * 256).rearrange("p (t c) -> p t c", t=NT); U_b = [Buf("pu%d" % i) for i in range(NT)]
    band = A.alloc(16 * 128).rearrange("p (g t) -> p g t", g=16); bband = Buf("band")
    dma(P, "sp", band, C.pband.rearrange("p (g t) -> p g t", g=16), bband, writes=[bband])
    pw = A.alloc(4 * 128, BF16).rearrange("p (g t) -> p g t", g=4); bpw = Buf("pw")
    dma(P, "pool", pw[0:64], C.pool_w.rearrange("p (g t) -> p g t", g=4), bpw, writes=[bpw])
    psc = A.alloc(2); bpsc = Buf("psc")
    for c in range(2):
        dma(P, "sp", psc[:, c:c + 1], C.pool_scale[:, c * 128:(c + 1) * 128].rearrange("a (p o) -> (a p) o", o=1), bpsc, writes=[bpsc])
    dltT = A.alloc(4 * 2048, BF16).rearrange("p (g t) -> p g t", g=4); dl_b = [Buf("dlt%d" % i) for i in range(4)]
    if True:
      proj_V(P, C, Wv, bWv, 0,
           [([C.hT_b[tl // 4]], (lambda k, tl=tl: C.hT[:, k, tl * 128:(tl + 1) * 128]), 128) for tl in range(NT)],
           [0, 1, 2, 3], lambda i: U32[:, i, :].rearrange("p (h d) -> p h d", h=4), lambda i: U_b[i])
    for j in range(16):
        dps, bd = bank(P, 4 + j % 2)
        rels = [(0, 3), (1, 2)] if j == 0 else [(-1, 0), (0, 1), (1, 2)]
        for g in range(4):
            for ri, (rel, v) in enumerate(rels):
                mm(P, dps[0:64, g * 128:(g + 1) * 128], U32[:, j + rel, g * 64:(g + 1) * 64], band[:, g * 4 + v, :],
                   start=(ri == 0), stop=(ri == len(rels) - 1), reads=[U_b[j + rel], bband], writes=[bd])
        cp(P, "act" if j % 2 == 0 else "dve", dltT[0:64, :, j * 128:(j + 1) * 128], dps[0:64, :].rearrange("p (g t) -> p g t", g=4),
           writes=[dl_b[j // 4], bd])
    for ch in range(2):
        for qc in range(4):
            yps, by = bank(P, 6 + qc % 2)
            mm(P, yps, pw[0:64, 2 * ch, :], dltT[0:64, 2 * ch, qc * 512:(qc + 1) * 512], start=True, stop=False, reads=[bpw, dl_b[qc]], writes=[by])
            mm(P, yps, pw[0:64, 2 * ch + 1, :], dltT[0:64, 2 * ch + 1, qc * 512:(qc + 1) * 512], start=False, stop=True, reads=[bpw, dl_b[qc]], writes=[by])
            ts(P, "dve", C.yT[:, 4 + ch, qc * 512:(qc + 1) * 512], yps, psc[:, ch:ch + 1], None, ALU.mult, reads=[bpsc], writes=[C.yT_b[2][qc], by])
    P.barrier()
    A.release(m0)


def pool_band(hf):
    W = (2, 4, 8, 16)
    out = np.zeros((128, 16, 128), np.float32)
    tl = np.arange(384)
    tg = tl if hf == 0 else (S - 1 - tl)
    for g, w in enumerate(W):
        A_ = np.zeros((384, 256), np.float32)
        for t in range(256):
            g_t = tg[t]
            lo = max(g_t - w // 2, 0); hi = min(g_t + w - 1 - w // 2, S - 1)
            cnt = hi - lo + 1
            inwin = (tg >= lo) & (tg <= hi)
            A_[inwin, t] += np.float32(1.0) / np.float32(cnt)
            A_[t, t] -= 1.0
        out[:, g * 4 + 0, :] = A_[0:128, 128:256]
        out[:, g * 4 + 1, :] = A_[128:256, 128:256]
        out[:, g * 4 + 2, :] = A_[256:384, 128:256]
        out[:, g * 4 + 3, :] = A_[0:128, 0:128]
    return out.reshape(128, 16 * 128)


def dil_masks():
    i = np.arange(128)[:, None]; a = np.arange(128)[None, :]
    mA = (i >= a).astype(np.float32); mB = (i <= a).astype(np.float32)
    mA0 = np.zeros((128, 128), np.float32); mA0[0:64] = mA[64:128]
    return np.concatenate([mA, mB, mA0], axis=1)


def pool_w_padded(pw):
    out = np.zeros((64, 4, 128), np.float32)
    for g in range(4):
        o = (g % 2) * 64
        out[:, g, o:o + 64] = pw[g]
    return out.reshape(64, 512)


def ln_tile(P, C, x, bx, h, bh, s, bs, gB, b_gB, bB, b_bB):
    stats = s[:, 0:12]; mv = s[:, 12:14]; rstd = s[:, 14:15]; nmr = s[:, 15:16]
    P.op("dve", lambda e: e.bn_stats(stats[:, 0:6], x[:, 0:512]), reads=[bx], writes=[bs])
    P.op("dve", lambda e: e.bn_stats(stats[:, 6:12], x[:, 512:1024]), reads=[bx], writes=[bs])
    P.op("dve", lambda e: e.bn_aggr(mv, stats.rearrange("p (c s) -> p c s", s=6)), reads=[bs], writes=[bs])
    act(P, rstd, mv[:, 1:2], AF.Sqrt, bias=C.eps, scale=1.0, reads=[bs, C.eps_b], writes=[bs])
    recip(P, rstd, rstd, reads=[bs], writes=[bs])
    stt(P, "dve", nmr, mv[:, 0:1], -1.0, rstd, ALU.mult, ALU.mult, reads=[bs], writes=[bs])
    act(P, h, x, AF.Identity, bias=nmr, scale=rstd, reads=[bx, bs], writes=[bh])
    tt(P, "pool", h, h, gB, ALU.mult, reads=[bh, b_gB], writes=[bh])
    tt(P, "dve", h, h, bB, ALU.add, reads=[bh, b_bB], writes=[bh])


def gate_phase(P, A, C):
    m0 = A.mark()
    A.limit = A.n - 16384
    mT = A.top_view(16384, 8 * 2048, BF16).rearrange("p (c t) -> p c t", c=8); mT_b = [Buf("mT%d" % i) for i in range(4)]
    bg = A.alloc(32); bbg = Buf("bgate")
    dma(P, "sp", bg, C.b_gate, bbg, writes=[bbg])
    m1 = A.mark()
    Wg = [A.alloc(8 * 4 * 128, BF16).rearrange("p (k n x) -> p k n x", k=8, n=4) for _ in range(2)]; Wg_b = [Buf("Wg%d" % i) for i in range(2)]
    Wb = [A.alloc(4 * 2 * 128, BF16).rearrange("p (n k x) -> p n k x", n=4, k=2) for _ in range(2)]; Wb_b = [Buf("Wb%d" % i) for i in range(2)]
    gsb = [A.alloc(512) for _ in range(3)]; gsb_b = [Buf() for _ in range(3)]
    tmp = [A.alloc(512) for _ in range(2)]; tmp_b = [Buf() for _ in range(2)]
    macc = [A.alloc(512) for _ in range(2)]; macc_b = [Buf() for _ in range(2)]
    gi = 0; ti = 0; bi = 0
    for c in range(8):
        wg = Wg[c % 2]; bwg = Wg_b[c % 2]; wb = Wb[c % 2]; bwb = Wb_b[c % 2]
        for n in range(4):
            col = (T_GATE + n * 8 + c) * 128
            dma(P, "pool", wg[:, :, n, :], C.w_in[:, col:col + 128].rearrange("(k p) x -> p k x", p=128), bwg, writes=[bwg])
            dma(P, "pool", wb[:, n, :, :], C.w_branch[n, :, c * 128:(c + 1) * 128].rearrange("(k p) x -> p k x", p=128), bwb, writes=[bwb])
        for tcq in range(4):
            ma = macc[(c * 4 + tcq) % 2]; bma = macc_b[(c * 4 + tcq) % 2]
            for n in range(4):
                gps, bgps = bank(P, bi % 4); pps, bpps = bank(P, 4 + bi % 4); bi += 1
                for k in range(8):
                    mm(P, gps, wg[:, k, n, :], C.hT[:, k, tcq * 512:(tcq + 1) * 512], start=(k == 0), stop=(k == 7),
                       reads=[bwg, C.hT_b[tcq]], writes=[bgps])
                for kc in range(2):
                    mm(P, pps, wb[:, n, kc, :], C.yT[:, n * 2 + kc, tcq * 512:(tcq + 1) * 512], start=(kc == 0), stop=(kc == 1),
                       reads=[bwb, C.yT_b[n][tcq]], writes=[bpps])
                g = gsb[gi % 3]; bgs_ = gsb_b[gi % 3]; gi += 1
                act(P, g, gps, AF.Sigmoid, bias=bg[:, n * 8 + c:n * 8 + c + 1], scale=1.0, reads=[bbg], writes=[bgs_, bgps])
                if n == 0:
                    tt(P, "dve", ma, g, pps, ALU.mult, reads=[bgs_], writes=[bma, bpps])
                else:
                    t_ = tmp[ti % 2]; bt_ = tmp_b[ti % 2]; ti += 1
                    tt(P, "dve", t_, g, pps, ALU.mult, reads=[bgs_], writes=[bt_, bpps])
                    if n < 3:
                        tt(P, "pool", ma, ma, t_, ALU.add, reads=[bt_], writes=[bma])
                    else:
                        tt(P, "pool", mT[:, c, tcq * 512:(tcq + 1) * 512], ma, t_, ALU.add, reads=[bt_, bma], writes=[mT_b[tcq]])
    P.barrier()
    A.release(m1)
    Wo = A.alloc(8 * 1024, BF16).rearrange("p (k c) -> p k c", k=8); bWo = Buf("Wo")
    dma(P, "pool", Wo, C.w_out.rearrange("(k p) c -> p k c", p=128), bWo, writes=[bWo])
    gB = A.alloc(1024); bB = A.alloc(1024); b_gB, b_bB = Buf("g1"), Buf("b1")
    dma(P, "sp", gB, C.ln1g.partition_broadcast(128), b_gB, writes=[b_gB])
    dma(P, "sp", bB, C.ln1b.partition_broadcast(128), b_bB, writes=[b_bB])
    Wr = A.alloc(8 * 36).rearrange("p (k c) -> p k c", k=8); bWr = Buf("Wr")
    dma(P, "sp", Wr, C.w_router.rearrange("(k p) c -> p k c", p=128), bWr, writes=[bWr])
    rb = A.alloc(36); brb = Buf("rbias")
    dma(P, "sp", rb, C.b_router.partition_broadcast(128), brb, writes=[brb])
    hs = [A.alloc(1024) for _ in range(2)]; hs_b = [Buf("h%d" % i) for i in range(2)]
    st = [A.alloc(16) for _ in range(2)]; st_b = [Buf() for _ in range(2)]
    hf32 = [A.alloc(1024).rearrange("p (k t) -> p k t", k=8) for _ in range(1)]; hf32_b = [Buf() for _ in range(1)]
    rs = [A.alloc(128) for _ in range(2)]; rs_b = [Buf() for _ in range(2)]
    BIG = 30000.0
    for t in range(16):
        h = hs[t % 2]; bh = hs_b[t % 2]; s = st[t % 2]; bs = st_b[t % 2]
        dma(P, "sp", h, C.hres[t * 128:(t + 1) * 128, :], bh, reads=[C.hres_b[t]], writes=[bh])
        for half in range(2):
            mps, bm = bank(P, (t % 2) * 2 + half)
            for k in range(8):
                mm(P, mps, mT[:, k, t * 128:(t + 1) * 128], Wo[:, k, half * 512:(half + 1) * 512], start=(k == 0), stop=(k == 7),
                   reads=[mT_b[t // 4], bWo], writes=[bm])
            stt(P, "dve", h[:, half * 512:(half + 1) * 512], h[:, half * 512:(half + 1) * 512], float(ALPHA), mps, ALU.mult, ALU.add,
                writes=[bh, bm])
        ln_tile(P, C, h, bh, h, bh, s, bs, gB, b_gB, bB, b_bB)
        dma(P, "sp", C.hres[t * 128:(t + 1) * 128, :], h, bh, reads=[bh], writes=[C.hres_b[t]])
        hf = hf32[0]; bhf = hf32_b[0]
        for half in range(2):
            pb, bpb = bank(P, 4 + half)
            for j in range(4):
                kc = half * 4 + j
                trp(P, pb[:, j * 128:(j + 1) * 128], h[:, kc * 128:(kc + 1) * 128], C.ident, reads=[bh, C.ident_b], writes=[bpb])
            cp(P, "act", C.h1T[:, half * 4:(half + 1) * 4, t * 128:(t + 1) * 128], pb.rearrange("p (j t) -> p j t", j=4),
               writes=[C.h1T_b[t // 4], bpb])
            cp(P, "dve", hf[:, half * 4:(half + 1) * 4, :], pb.rearrange("p (j t) -> p j t", j=4), writes=[bhf, bpb])
        rps, brp = bank(P, 6)
        for k in range(8):
            mm(P, rps[:, 0:36], hf[:, k, :], Wr[:, k, :], start=(k == 0), stop=(k == 7), reads=[bhf, bWr], writes=[brp])
        r = rs[t % 2]; br = rs_b[t % 2]
        lg = r[:, 0:36]; gmx = r[:, 36:37]; oh = r[:, 40:44]; eg = r[:, 44:48]; sg = r[:, 48:49]; pen = r[:, 52:56]
        em = r[:, 56:88]; top = r[:, 88:96]; w = r[:, 96:128]
        tt(P, "dve", lg, rps[:, 0:36], rb, ALU.add, reads=[brb], writes=[br, brp])
        P.op("dve", lambda e, gmx=gmx, lg=lg: e.reduce_max(gmx, lg[:, 0:4], axis=AX.X), writes=[br])
        ts(P, "dve", oh, lg[:, 0:4], gmx, None, ALU.is_ge, writes=[br])
        ts(P, "dve", sg, gmx, -1.0, None, ALU.mult, writes=[br])
        act(P, eg, lg[:, 0:4], AF.Exp, bias=sg, scale=1.0, writes=[br])
        P.op("dve", lambda e, sg=sg, eg=eg: e.reduce_sum(sg, eg, axis=AX.X), writes=[br])
        recip(P, sg, sg, writes=[br])
        ts(P, "dve", pen, oh, BIG, -BIG, ALU.mult, ALU.add, writes=[br])
        tt(P, "dve", em.rearrange("p (g j) -> p g j", g=4), lg[:, 4:36].rearrange("p (g j) -> p g j", g=4),
           pen.unsqueeze(2).broadcast_to([128, 4, 8]), ALU.add, writes=[br])
        P.op("dve", lambda e, top=top, em=em: e.max(top, em), writes=[br])
        Gt = C.G[:, t, :]
        ts(P, "dve", Gt, em, top[:, 1:2], None, ALU.is_ge, writes=[br, C.G_b[t]])
        ts(P, "dve", gmx, top[:, 0:1], -1.0, None, ALU.mult, writes=[br])
        act(P, w, em, AF.Exp, bias=gmx, scale=1.0, writes=[br])
        tt(P, "dve", w, w, Gt, ALU.mult, writes=[br])
        P.op("dve", lambda e, gmx=gmx, w=w: e.reduce_sum(gmx, w, axis=AX.X), writes=[br])
        recip(P, gmx, gmx, writes=[br])
        tt(P, "dve", gmx, gmx, sg, ALU.mult, writes=[br])
        ts(P, "dve", Gt, w, gmx, None, ALU.mult, writes=[br, C.G_b[t]])
    P.barrier()
    A.release(m0)


def moe_phase(P, A, C, final_out, final_bufs):
    m0 = A.mark()
    A.limit = A.n - 8192
    acc = A.alloc(16 * 1024).rearrange("p (t c) -> p t c", t=16); acc_b = [Buf("macc%d" % i) for i in range(16)]
    NW_ = 2
    Wg = [A.alloc(8 * 512, BF16).rearrange("p (k c) -> p k c", k=8) for _ in range(NW_)]
    Wu = [A.alloc(8 * 512, BF16).rearrange("p (k c) -> p k c", k=8) for _ in range(NW_)]
    Wd = [A.alloc(4 * 1024, BF16).rearrange("p (k c) -> p k c", k=4) for _ in range(NW_)]
    W_b = [[Buf("eW%d_%d" % (i, j)) for j in range(3)] for i in range(NW_)]
    AT = [A.alloc(4 * 512, BF16).rearrange("p (h t) -> p h t", h=4) for _ in range(2)]; AT_b = [Buf() for _ in range(2)]
    sg = [A.alloc(512) for _ in range(3)]; sg_b = [Buf() for _ in range(3)]
    si = 0; bi = 0; yi = 0
    for e in range(32):
        wg, wu, wd = Wg[e % NW_], Wu[e % NW_], Wd[e % NW_]; bw = W_b[e % NW_]
        dma(P, "pool", wg, C.ew_gate[e].rearrange("(k p) c -> p k c", p=128), bw[0], writes=[bw[0]])
        dma(P, "pool", wu, C.ew_up[e].rearrange("(k p) c -> p k c", p=128), bw[1], writes=[bw[1]])
        dma(P, "pool", wd, C.ew_down[e].rearrange("(k p) c -> p k c", p=128), bw[2], writes=[bw[2]])
        for tcq in range(4):
            at = AT[(e * 4 + tcq) % 2]; bat = AT_b[(e * 4 + tcq) % 2]
            for hc in range(4):
                gps, bg_ = bank(P, (bi % 3) * 2); ups, bu_ = bank(P, (bi % 3) * 2 + 1); bi += 1
                for k in range(8):
                    mm(P, gps, wg[:, k, hc * 128:(hc + 1) * 128], C.h1T[:, k, tcq * 512:(tcq + 1) * 512], start=(k == 0), stop=(k == 7),
                       reads=[bw[0], C.h1T_b[tcq]], writes=[bg_])
                for k in range(8):
                    mm(P, ups, wu[:, k, hc * 128:(hc + 1) * 128], C.h1T[:, k, tcq * 512:(tcq + 1) * 512], start=(k == 0), stop=(k == 7),
                       reads=[bw[1], C.h1T_b[tcq]], writes=[bu_])
                s_ = sg[si % 3]; bs_ = sg_b[si % 3]; si += 1
                act(P, s_, gps, AF.Silu, writes=[bs_, bg_])
                tt(P, "dve", at[:, hc, :], s_, ups, ALU.mult, reads=[bs_], writes=[bat, bu_])
            for tq in range(4):
                t = tcq * 4 + tq
                for half in range(2):
                    yps, by = bank(P, 6 + yi % 2); yi += 1
                    for hc in range(4):
                        mm(P, yps, at[:, hc, tq * 128:(tq + 1) * 128], wd[:, hc, half * 512:(half + 1) * 512], start=(hc == 0), stop=(hc == 3),
                           reads=[bat, bw[2]], writes=[by])
                    dst = acc[:, t, half * 512:(half + 1) * 512]
                    if e == 0:
                        ts(P, "dve", dst, yps, C.G[:, t, e:e + 1], None, ALU.mult, reads=[C.G_b[t]], writes=[acc_b[t], by])
                    else:
                        stt(P, "dve", dst, yps, C.G[:, t, e:e + 1], dst, ALU.mult, ALU.add, reads=[C.G_b[t]], writes=[acc_b[t], by])
    gB = A.alloc(1024); bB = A.alloc(1024); b_gB, b_bB = Buf("g2"), Buf("b2")
    dma(P, "sp", gB, C.ln2g.partition_broadcast(128), b_gB, writes=[b_gB])
    dma(P, "sp", bB, C.ln2b.partition_broadcast(128), b_bB, writes=[b_bB])
    hs = [A.alloc(1024) for _ in range(2)]; hs_b = [Buf("h2_%d" % i) for i in range(2)]
    st = [A.alloc(16) for _ in range(2)]; st_b = [Buf() for _ in range(2)]
    for t in range(16):
        h = hs[t % 2]; bh = hs_b[t % 2]; s = st[t % 2]; bs = st_b[t % 2]
        dma(P, "sp", h, C.hres[t * 128:(t + 1) * 128, :], bh, reads=[C.hres_b[t]], writes=[bh])
        stt(P, "dve", h, h, float(ALPHA), acc[:, t, :], ALU.mult, ALU.add, reads=[acc_b[t]], writes=[bh])
        ln_tile(P, C, h, bh, h, bh, s, bs, gB, b_gB, bB, b_bB)
        dma(P, "sp", final_out[t * 128:(t + 1) * 128, :], h, bh, reads=[bh], writes=[final_bufs[t]])
    P.barrier()
    A.release(m0)


def declare_layer_inputs(nc, C, sfx=""):
    def inp(name, shape):
        return nc.dram_tensor(name + sfx, shape, F32, kind="ExternalInput").ap()
    L = Ctx()
    L.w_in = inp("w_in_r", [1024, NCOL])
    L.na_tab = inp("na_tab", [128, 7680])
    L.diff_lam = inp("diff_lam", [1, 128])
    L.diff_g = inp("diff_g", [1, 64])
    L.pool_w = inp("pool_w", [64, 512])
    L.pool_scale = inp("pool_scale", [1, 256])
    L.b_gate = inp("b_gate_r", [128, 32])
    L.w_branch = inp("w_branch", [4, 256, 1024])
    L.w_out = inp("w_out", [1024, 1024])
    L.ln1g = inp("ln1g", [1, 1024]); L.ln1b = inp("ln1b", [1, 1024])
    L.w_router = inp("w_router", [1024, 36]); L.b_router = inp("b_router", [1, 36])
    L.ew_gate = inp("ew_gate", [32, 1024, 512]); L.ew_up = inp("ew_up", [32, 1024, 512]); L.ew_down = inp("ew_down", [32, 512, 1024])
    L.ln2g = inp("ln2g", [1, 1024]); L.ln2b = inp("ln2b", [1, 1024])
    return L


def host_layer_inputs(d, l, hf, sfx=""):
    r = {
        "w_in_r": relayout_w_in(d["w_in"][l]),
        "na_tab": na_tables(hf, d["na_rpb"][l]),
        "diff_lam": np.ascontiguousarray(d["diff_lam"][l].reshape(1, 128)),
        "diff_g": np.ascontiguousarray(d["diff_subln_g"][l][None, :]),
        "pool_w": pool_w_padded(d["pool_w"][l]),
        "pool_scale": np.ascontiguousarray(d["pool_scale"][l][None, :]),
        "b_gate_r": np.ascontiguousarray(d["b_gate"][l].reshape(32, 128).T),
        "w_branch": np.ascontiguousarray(d["w_branch"][l]),
        "w_out": np.ascontiguousarray(d["w_out"][l]),
        "ln1g": np.ascontiguousarray(d["ln1_g"][l][None, :]), "ln1b": np.ascontiguousarray(d["ln1_b"][l][None, :]),
        "w_router": np.ascontiguousarray(np.concatenate([d["router_group_w"][l], d["router_expert_w"][l]], axis=1)),
        "b_router": np.ascontiguousarray(np.concatenate([d["router_group_b"][l], d["router_expert_b"][l]])[None, :]),
        "ew_gate": np.ascontiguousarray(d["expert_w_gate"][l]), "ew_up": np.ascontiguousarray(d["expert_w_up"][l]),
        "ew_down": np.ascontiguousarray(d["expert_w_down"][l]),
        "ln2g": np.ascontiguousarray(d["ln2_g"][l][None, :]), "ln2b": np.ascontiguousarray(d["ln2_b"][l][None, :]),
    }
    return {k + sfx: v for k, v in r.items()}


def declare_shared_inputs(nc, C):
    def inp(name, shape):
        return nc.dram_tensor(name, shape, F32, kind="ExternalInput").ap()
    C.ident_d = inp("ident", [128, 128])
    C.rope_d = inp("rope_d", [2, 128, 4096]); C.rope_l = inp("rope_l", [2, 128, 4096])
    C.dmask = inp("dmask", [128, 384]); C.pband = inp("pband", [128, 2048])
    C.lng = inp("lng", [1, 1024]); C.lnb = inp("lnb", [1, 1024])


def host_shared_inputs(d, hf):
    return {"ident": np.eye(128, dtype=np.float32), "rope_d": rope_tables(hf, 8, 32), "rope_l": rope_tables(hf, 16, 64),
            "dmask": dil_masks(), "pband": pool_band(hf),
            "lng": np.ascontiguousarray(d["emb_ln_g"][None, :]), "lnb": np.ascontiguousarray(d["emb_ln_b"][None, :])}


def layer_program(P, A, C, L, l, do_ln, final_out, final_bufs, upto=99):
    for k, v in L.__dict__.items():
        setattr(C, k, v)
    m = A.mark()
    A.limit = A.n
    C.hT = A.alloc(8 * 4096, BF16).rearrange("p (k t) -> p k t", k=8)
    C.hT_b = [Buf("hT%d" % i) for i in range(8)]
    C.yT = A.alloc(8 * 2048, BF16).rearrange("p (c t) -> p c t", c=8)
    C.yT_b = [[Buf("yT%d_%d" % (mx, g)) for g in range(4)] for mx in range(4)]
    stage_ln(P, A, C, do_ln)
    P.barrier()
    mixer_na(P, A, C); P.barrier()
    mixer_diff(P, A, C, 0.8 - 0.6 * math.exp(-0.3 * l))
    mixer_dil(P, A, C)
    mixer_pool(P, A, C)
    if upto >= 1:
        gate_phase(P, A, C)
    P.barrier()
    A.release(m)
    if upto >= 2:
        moe_phase(P, A, C, final_out, final_bufs)


def setup_persistent(P, A, C):
    setup_common(P, A, C, P.nc)
    C.G = A.alloc(16 * 32).rearrange("p (t e) -> p t e", t=16); C.G_b = [Buf("G%d" % i) for i in range(16)]
    C.h1T = A.top_view(8192, 8 * 2048, BF16).rearrange("p (k t) -> p k t", k=8); C.h1T_b = [Buf("h1T%d" % i) for i in range(4)]


def build_layer_nc(l, do_ln):
    nc = bass.Bass("TRN2", target_bir_lowering=False)
    es = ExitStack()
    C = Ctx()
    C.xin = nc.dram_tensor("xin", [4096, 1024], F32, kind="ExternalInput").ap()
    declare_shared_inputs(nc, C)
    L = declare_layer_inputs(nc, C)
    hout = nc.dram_tensor("hout", [2048, 1024], F32, kind="ExternalOutput").ap()
    C.hres = nc.dram_tensor("hres", [2048, 1024], F32, kind="Internal").ap()
    with es:
        NW = 51 * 1024
        arena_t = es.enter_context(nc.sbuf_tensor("arena", [128, NW], F32))
        A = Arena(arena_t[:], NW)
        P = Prog(nc, es)
        P.psum = [es.enter_context(nc.psum_tensor("ps%d" % i, [128, 512], F32))[:] for i in range(8)]
        P.psb = [Buf("ps%d" % i) for i in range(8)]
        setup_persistent(P, A, C)
        C.hres_b = [Buf("hres%d" % i) for i in range(16)]
        hout_b = [Buf("hout%d" % i) for i in range(16)]
        layer_program(P, A, C, L, l, do_ln, hout, hout_b)
        P.wait_all("sp", hout_b)
        P.emit()
    return nc


def to_local(a, hf):
    return np.ascontiguousarray(a if hf == 0 else a[::-1])


def kernel(**inputs):
    d = {k: np.asarray(v) for k, v in inputs.items()}
    cur = d["x"].astype(np.float32)
    for l in range(2):
        nc = build_layer_nc(l, l == 0)
        in_maps = []
        for c in range(8):
            b, hf = divmod(c, 2)
            m = {"xin": to_local(cur[b], hf)}
            m.update(host_shared_inputs(d, hf))
            m.update(host_layer_inputs(d, l, hf))
            in_maps.append(m)
        res = run_bass_kernel_spmd(nc, in_maps, core_ids=list(range(8)))
        nxt = np.empty_like(cur)
        for c in range(8):
            b, hf = divmod(c, 2)
            own = np.asarray(res.results[c]["hout"])
            if hf == 0:
                nxt[b, 0:2048] = own
            else:
                nxt[b, 2048:4096] = own[::-1]
        cur = nxt
    return cur
```
